# Optimizing a Trainium2 kernel written in Bass

```python
import jax, jax.numpy as jnp
from jax import lax
import numpy as np

D_MODEL = 2048
BATCH = 16
SEQ = 256
DEPTH = 1
DEC_BATCH = 2
DEC_SEQ = 4096
PAST_LEN = 512

GRID_W = 64
N_HEADS = 16
N_KV_HEADS = 4
HEAD_DIM = 128
GROUP = N_HEADS // N_KV_HEADS
ATTN_WIDTH = N_HEADS * HEAD_DIM
KV_WIDTH = N_KV_HEADS * HEAD_DIM
WINDOW = 128
BLOCK = 128
CONV_WIDTH = D_MODEL
CONV_K = 3
N_EXPERTS = 64
TOP_K = 8
N_EXPERT_GROUPS = 8
TOPK_GROUPS = 4
D_EXPERT = 512
D_SHARED = 512
ROUTED_SCALE = 2.5
MOE_CHUNK = 128
ROPE_THETA = 10000.0
EPS = 1e-6
NEG_INF = -1e30
ATTN_SCALE = HEAD_DIM ** -0.5
IN_WIDTHS = (CONV_WIDTH, CONV_WIDTH, CONV_WIDTH, ATTN_WIDTH, KV_WIDTH, KV_WIDTH, D_MODEL, D_MODEL)
IN_WIDTH = sum(IN_WIDTHS)
IN_SPLITS = tuple(int(s) for s in np.cumsum(IN_WIDTHS)[:-1])

kernel_name = "hybrid_dit_conv_swa_moe_step"


def rmsnorm(x, g):
    xf = x.astype(jnp.float32)
    y = xf * lax.rsqrt(jnp.mean(xf * xf, axis=-1, keepdims=True) + EPS)
    return (y * g.astype(jnp.float32)).astype(x.dtype)


def adaln_mods(cond, w_ada, b_ada):
    m = (jax.nn.silu(cond) @ w_ada + b_ada)[:, None, :]
    return jnp.split(m, 6, axis=-1)


def modulate(h, shift, scale):
    return h * (1.0 + scale) + shift


def centred_short_conv(u, w):
    up = jnp.pad(u, ((0, 0), (1, 1), (0, 0)))
    return up[:, :-2] * w[0] + up[:, 1:-1] * w[1] + up[:, 2:] * w[2]


def axial_rope_tables(n_tokens):
    rows = n_tokens // GRID_W
    row = jnp.repeat(jnp.arange(rows, dtype=jnp.float32), GRID_W)
    col = jnp.tile(jnp.arange(GRID_W, dtype=jnp.float32), rows)
    n_freq = HEAD_DIM // 4
    inv = ROPE_THETA ** (-jnp.arange(n_freq, dtype=jnp.float32) / n_freq)
    ang = jnp.concatenate([row[:, None] * inv, col[:, None] * inv], axis=-1)
    return jnp.cos(ang)[:, None, :], jnp.sin(ang)[:, None, :]


def apply_rope(x, cos, sin):
    xf = x.astype(jnp.float32)
    x1, x2 = xf[..., :HEAD_DIM // 2], xf[..., HEAD_DIM // 2:]
    return jnp.concatenate([x1 * cos - x2 * sin, x1 * sin + x2 * cos], axis=-1).astype(x.dtype)


def sink_softmax(s, sink):
    sk = sink.astype(jnp.float32).reshape(N_KV_HEADS, GROUP)[None, :, :, None, None]
    m = jnp.maximum(jnp.max(s, axis=-1, keepdims=True), sk)
    p = jnp.exp(s - m)
    return p / (jnp.sum(p, axis=-1, keepdims=True) + jnp.exp(sk - m))


def context_attention(q, k, v, sink):
    B, S = q.shape[0], q.shape[1]
    nb = S // BLOCK
    qb = jnp.moveaxis(q.reshape(B, nb, BLOCK, N_KV_HEADS, GROUP, HEAD_DIM), 1, 0)
    kf = k.astype(jnp.float32)
    vf = v.astype(jnp.float32)

    def one(qblk):
        s = jnp.einsum('bqkgd,bskd->bkgqs', qblk.astype(jnp.float32), kf) * ATTN_SCALE
        p = sink_softmax(s, sink)
        return jnp.einsum('bkgqs,bskd->bqkgd', p, vf)

    o = lax.map(one, qb)
    return jnp.moveaxis(o, 0, 1).reshape(B, S, ATTN_WIDTH).astype(q.dtype)


def latent_attention(q, k, v, kc, vc, sink):
    B, N = q.shape[0], q.shape[1]
    nb = N // BLOCK
    qb = jnp.moveaxis(q.reshape(B, nb, BLOCK, N_KV_HEADS, GROUP, HEAD_DIM), 1, 0)

    def band(t):
        tp = jnp.pad(t.astype(jnp.float32), ((0, 0), (BLOCK, BLOCK), (0, 0), (0, 0)))
        tp = tp.reshape(B, nb + 2, BLOCK, N_KV_HEADS, HEAD_DIM)
        bt = jnp.concatenate([tp[:, :-2], tp[:, 1:-1], tp[:, 2:]], axis=2)
        return jnp.moveaxis(bt, 1, 0)

    kb, vb = band(k), band(v)
    qpos = jnp.arange(nb)[:, None] * BLOCK + jnp.arange(BLOCK)[None, :]
    kpos = jnp.arange(nb)[:, None] * BLOCK - BLOCK + jnp.arange(3 * BLOCK)[None, :]
    valid = ((jnp.abs(qpos[:, :, None] - kpos[:, None, :]) <= WINDOW)
             & (kpos[:, None, :] >= 0) & (kpos[:, None, :] < N))
    kcf = kc.astype(jnp.float32)
    vcf = vc.astype(jnp.float32)

    def one(args):
        qblk, kblk, vblk, vmask = args
        qf = qblk.astype(jnp.float32)
        s_loc = jnp.einsum('bqkgd,bskd->bkgqs', qf, kblk) * ATTN_SCALE
        s_loc = jnp.where(vmask[None, None, None], s_loc, NEG_INF)
        s_ctx = jnp.einsum('bqkgd,bpkd->bkgqp', qf, kcf) * ATTN_SCALE
        p = sink_softmax(jnp.concatenate([s_loc, s_ctx], axis=-1), sink)
        return (jnp.einsum('bkgqs,bskd->bqkgd', p[..., :3 * BLOCK], vblk)
                + jnp.einsum('bkgqp,bpkd->bqkgd', p[..., 3 * BLOCK:], vcf))

    o = lax.map(one, (qb, kb, vb, valid))
    return jnp.moveaxis(o, 0, 1).reshape(B, N, ATTN_WIDTH).astype(q.dtype)


def mixer_inputs(h, w_in, conv_w, w_conv_out, q_norm_g, k_norm_g):
    B, S = h.shape[0], h.shape[1]
    cb, cc, cx, q, k, v, g_conv, g_attn = jnp.split(h @ w_in, IN_SPLITS, axis=-1)
    y_conv = (cb * centred_short_conv(cc * cx, conv_w)) @ w_conv_out
    q = rmsnorm(q.reshape(B, S, N_HEADS, HEAD_DIM), q_norm_g)
    k = rmsnorm(k.reshape(B, S, N_KV_HEADS, HEAD_DIM), k_norm_g)
    v = v.reshape(B, S, N_KV_HEADS, HEAD_DIM)
    return y_conv, q, k, v, g_conv, g_attn


def merge_branches(y_conv, attn, g_conv, g_attn, w_attn_out, w_out):
    y_attn = attn @ w_attn_out
    return (jax.nn.sigmoid(g_conv) * y_conv + jax.nn.sigmoid(g_attn) * y_attn) @ w_out


def moe_ffn(h, router_w, router_bias, w_eg, w_eu, w_ed, w_sg, w_su, w_sd):
    B, S, D = h.shape
    t = h.reshape(B * S, D)
    T = t.shape[0]
    scores = jax.nn.sigmoid(t.astype(jnp.float32) @ router_w.astype(jnp.float32))
    biased = scores + router_bias.astype(jnp.float32)
    grp = biased.reshape(T, N_EXPERT_GROUPS, N_EXPERTS // N_EXPERT_GROUPS)
    grp_score = jnp.sum(lax.top_k(grp, 2)[0], axis=-1)
    _, gidx = lax.top_k(grp_score, TOPK_GROUPS)
    gsel = jnp.any(jax.nn.one_hot(gidx, N_EXPERT_GROUPS, dtype=jnp.float32) > 0, axis=1)
    emask = jnp.repeat(gsel, N_EXPERTS // N_EXPERT_GROUPS, axis=1)
    _, eidx = lax.top_k(jnp.where(emask, biased, NEG_INF), TOP_K)
    sel = jnp.take_along_axis(scores, eidx, axis=-1)
    wts = sel / jnp.sum(sel, axis=-1, keepdims=True) * ROUTED_SCALE
    dense_w = jnp.sum(jax.nn.one_hot(eidx, N_EXPERTS, dtype=jnp.float32) * wts[..., None], axis=1)
    nch = T // MOE_CHUNK
    tc = t.reshape(nch, MOE_CHUNK, D)
    wc = dense_w.reshape(nch, MOE_CHUNK, N_EXPERTS).astype(t.dtype)

    def experts(args):
        xc, gw = args
        a = jax.nn.silu(jnp.einsum('cd,edf->cef', xc, w_eg)) * jnp.einsum('cd,edf->cef', xc, w_eu)
        return jnp.einsum('cef,efd->cd', a * gw[..., None], w_ed)

    routed = lax.map(experts, (tc, wc)).reshape(T, D)
    shared = (jax.nn.silu(t @ w_sg) * (t @ w_su)) @ w_sd
    return (routed + shared).reshape(B, S, D)


def setup_inputs(seed: int = 0) -> dict:
    key = jax.random.key(seed)
    ks = jax.random.split(key, 26)
    f32 = jnp.float32
    D = D_MODEL

    def nrm(k, shape, scale):
        return jax.random.normal(k, shape, f32) * scale

    return {
        "x_prompt": nrm(ks[0], (BATCH, SEQ, D), 1.0),
        "x_sample": nrm(ks[1], (DEC_BATCH, DEC_SEQ, D), 1.0),
        "cache_k": nrm(ks[2], (DEC_BATCH, DEPTH, PAST_LEN, N_KV_HEADS, HEAD_DIM), 1.0),
        "cache_v": nrm(ks[3], (DEC_BATCH, DEPTH, PAST_LEN, N_KV_HEADS, HEAD_DIM), 1.0),
        "c": nrm(ks[4], (DEC_BATCH, D), 1.0),
        "c_ctx": nrm(ks[5], (D,), 1.0),
        "w_ada": nrm(ks[6], (DEPTH, D, 6 * D), 0.5 * D ** -0.5),
        "b_ada": nrm(ks[7], (DEPTH, 6 * D), 0.02),
        "norm_mix_g": 1.0 + nrm(ks[8], (DEPTH, D), 0.02),
        "norm_ffn_g": 1.0 + nrm(ks[9], (DEPTH, D), 0.02),
        "w_in": nrm(ks[10], (DEPTH, D, IN_WIDTH), D ** -0.5),
        "conv_w": nrm(ks[11], (DEPTH, CONV_K, CONV_WIDTH), CONV_K ** -0.5),
        "q_norm_g": 1.0 + nrm(ks[12], (DEPTH, HEAD_DIM), 0.02),
        "k_norm_g": 1.0 + nrm(ks[13], (DEPTH, HEAD_DIM), 0.02),
        "attn_sink": nrm(ks[14], (DEPTH, N_HEADS), 0.5),
        "w_conv_out": nrm(ks[15], (DEPTH, CONV_WIDTH, D), CONV_WIDTH ** -0.5),
        "w_attn_out": nrm(ks[16], (DEPTH, ATTN_WIDTH, D), ATTN_WIDTH ** -0.5),
        "w_out": nrm(ks[17], (DEPTH, D, D), D ** -0.5),
        "router_w": nrm(ks[18], (DEPTH, D, N_EXPERTS), D ** -0.5),
        "router_bias": nrm(ks[19], (DEPTH, N_EXPERTS), 0.01),
        "w_exp_gate": nrm(ks[20], (DEPTH, N_EXPERTS, D, D_EXPERT), D ** -0.5),
        "w_exp_up": nrm(ks[21], (DEPTH, N_EXPERTS, D, D_EXPERT), D ** -0.5),
        "w_exp_down": nrm(ks[22], (DEPTH, N_EXPERTS, D_EXPERT, D), D_EXPERT ** -0.5),
        "w_sh_gate": nrm(ks[23], (DEPTH, D, D_SHARED), D ** -0.5),
        "w_sh_up": nrm(ks[24], (DEPTH, D, D_SHARED), D ** -0.5),
        "w_sh_down": nrm(ks[25], (DEPTH, D_SHARED, D), D_SHARED ** -0.5),
    }


def reference(x_prompt, x_sample, cache_k, cache_v, c, c_ctx, w_ada, b_ada, norm_mix_g, norm_ffn_g,
              w_in, conv_w, q_norm_g, k_norm_g, attn_sink, w_conv_out, w_attn_out, w_out,
              router_w, router_bias, w_exp_gate, w_exp_up, w_exp_down, w_sh_gate, w_sh_up, w_sh_down):
    cos, sin = axial_rope_tables(x_sample.shape[1])
    xp = x_prompt
    xs = x_sample
    ks_new = []
    vs_new = []
    for l in range(DEPTH):
        sh_a, sc_a, gt_a, sh_m, sc_m, gt_m = adaln_mods(c_ctx[None, :], w_ada[l], b_ada[l])
        h = modulate(rmsnorm(xp, norm_mix_g[l]), sh_a, sc_a)
        y_conv, q, k, v, g_conv, g_attn = mixer_inputs(h, w_in[l], conv_w[l], w_conv_out[l], q_norm_g[l], k_norm_g[l])
        attn = context_attention(q, k, v, attn_sink[l])
        xp = xp + gt_a * merge_branches(y_conv, attn, g_conv, g_attn, w_attn_out[l], w_out[l])
        h = modulate(rmsnorm(xp, norm_ffn_g[l]), sh_m, sc_m)
        xp = xp + gt_m * moe_ffn(h, router_w[l], router_bias[l], w_exp_gate[l], w_exp_up[l], w_exp_down[l],
                                 w_sh_gate[l], w_sh_up[l], w_sh_down[l])
        ks_new.append(k)
        vs_new.append(v)

        sh_a, sc_a, gt_a, sh_m, sc_m, gt_m = adaln_mods(c, w_ada[l], b_ada[l])
        h = modulate(rmsnorm(xs, norm_mix_g[l]), sh_a, sc_a)
        y_conv, q, k, v, g_conv, g_attn = mixer_inputs(h, w_in[l], conv_w[l], w_conv_out[l], q_norm_g[l], k_norm_g[l])
        q = apply_rope(q, cos, sin)
        k = apply_rope(k, cos, sin)
        attn = latent_attention(q, k, v, cache_k[:, l], cache_v[:, l], attn_sink[l])
        xs = xs + gt_a * merge_branches(y_conv, attn, g_conv, g_attn, w_attn_out[l], w_out[l])
        h = modulate(rmsnorm(xs, norm_ffn_g[l]), sh_m, sc_m)
        xs = xs + gt_m * moe_ffn(h, router_w[l], router_bias[l], w_exp_gate[l], w_exp_up[l], w_exp_down[l],
                                 w_sh_gate[l], w_sh_up[l], w_sh_down[l])
    new_k = jnp.stack(ks_new, axis=1)
    new_v = jnp.stack(vs_new, axis=1)
    return (xp, xs, new_k, new_v)
```

```python
import numpy as np
import concourse.bass as bass
import concourse.mybir as mybir
from concourse.bass_utils import run_bass_kernel_spmd

F32 = mybir.dt.float32
BF16 = mybir.dt.bfloat16
ALU = mybir.AluOpType
AF = mybir.ActivationFunctionType
AX = mybir.AxisListType
I32 = mybir.dt.int32
ESZ = {F32: 4, BF16: 2, I32: 4}
CAP = 320

D = 2048
NH, NKV, HD = 16, 4, 128
NE, DE = 64, 512
EPS = 1e-6
IN_W = 13312
OFF_CB, OFF_CC, OFF_CX, OFF_Q, OFF_K, OFF_V, OFF_GC, OFF_GA = 0, 2048, 4096, 6144, 8192, 8704, 9216, 11264
BIG = 1.0e30
NCORES = 8
NSL = NE * CAP


def region(ap):
    es = ESZ.get(ap.dtype, 4)
    pairs = ap.ap
    sp = str(ap.space).upper()
    if ("SBUF" in sp) or ("PSUM" in sp) or ("SB" == sp):
        free = 1
        for d in ap.tensor.shape[1:]:
            free *= d
        lo = ap.offset - ap.start_partition() * free
        span = sum((c - 1) * abs(s) for s, c in pairs[1:])
    else:
        lo = ap.offset
        span = sum((c - 1) * abs(s) for s, c in pairs)
    return (ap.name, lo * es, (lo + span + 1) * es)


class Tracker:
    ENGS = ("pe", "act", "dve", "pool", "sp")

    def __init__(self):
        self.ops = []
        self.recs = {}
        self.chan_last = {}
        self.nchan = 0
        self.groups = {}

    def new_chans(self, group, n):
        self.groups[group] = [list(range(self.nchan, self.nchan + n)), 0]
        self.nchan += n

    def add(self, eng, fn, ins, outs, chan_group=None, extra=()):
        idx = len(self.ops)
        deps = set(extra)
        chan = None
        if chan_group is not None:
            g = self.groups[chan_group]
            chan = g[0][g[1] % len(g[0])]
            g[1] += 1
            if chan in self.chan_last:
                deps.add(self.chan_last[chan])
            self.chan_last[chan] = idx
        key = ("c", chan) if chan is not None else eng
        for ap in ins:
            if ap is None:
                continue
            name, lo, hi = region(ap)
            lst = self.recs.setdefault(name, [])
            found = False
            for r in lst:
                if r[0] < hi and lo < r[1]:
                    if r[2]:
                        deps.add(r[4])
                    elif r[0] == lo and r[1] == hi and r[3] == key:
                        r[4] = idx
                        found = True
            if not found:
                lst.append([lo, hi, False, key, idx])
        for ap in outs:
            name, lo, hi = region(ap)
            lst = self.recs.setdefault(name, [])
            keep = []
            for r in lst:
                if r[0] < hi and lo < r[1]:
                    if r[4] != idx:
                        deps.add(r[4])
                    if lo <= r[0] and r[1] <= hi:
                        continue
                keep.append(r)
            keep.append([lo, hi, True, key, idx])
            self.recs[name] = keep
        deps.discard(idx)
        self.ops.append({"eng": eng, "fn": fn, "deps": deps, "chan": chan})
        return idx

    def emit(self, nc):
        ops = self.ops
        marked = [False] * len(ops)
        for i, o in enumerate(ops):
            if o["chan"] is not None:
                marked[i] = True
            for d in o["deps"]:
                po = ops[d]
                if po["chan"] is None and po["eng"] == "pe" and o["eng"] == "pe" and o["chan"] is None:
                    continue
                marked[d] = True
        cnt = {}
        val = [0] * len(ops)
        for i, o in enumerate(ops):
            if not marked[i]:
                continue
            key = ("c", o["chan"]) if o["chan"] is not None else o["eng"]
            inc = 16 if o["chan"] is not None else 1
            cnt[key] = cnt.get(key, 0) + inc
            val[i] = cnt[key]
        self.final_counts = dict(cnt)
        per_eng = {e: [] for e in self.ENGS}
        for i, o in enumerate(ops):
            per_eng[o["eng"]].append(i)
        import contextlib
        with contextlib.ExitStack() as st:
            sems = {}
            for e in self.ENGS:
                sems[e] = st.enter_context(nc.semaphore("s_" + e))
            for c in range(self.nchan):
                sems[("c", c)] = st.enter_context(nc.semaphore("s_c%d" % c))
            block = st.enter_context(nc.Block())

            def run_engine(ename, e):
                seen = {}
                for i in per_eng[ename]:
                    o = ops[i]
                    waits = {}
                    for d in o["deps"]:
                        po = ops[d]
                        pkey = ("c", po["chan"]) if po["chan"] is not None else po["eng"]
                        if pkey == "pe" and ename == "pe" and o["chan"] is None:
                            continue
                        v = val[d]
                        if v > waits.get(pkey, 0):
                            waits[pkey] = v
                    for pkey, v in waits.items():
                        if seen.get(pkey, 0) >= v:
                            continue
                        seen[pkey] = v
                        e.wait_ge(sems[pkey], v)
                    ins = o["fn"](e)
                    if marked[i]:
                        key = ("c", o["chan"]) if o["chan"] is not None else o["eng"]
                        ins.then_inc(sems[key], 16 if o["chan"] is not None else 1)
                if ename == "sp":
                    for key, v in self.final_counts.items():
                        if seen.get(key, 0) < v:
                            e.wait_ge(sems[key], v)

            block.tensor(lambda e: run_engine("pe", e))
            block.scalar(lambda e: run_engine("act", e))
            block.vector(lambda e: run_engine("dve", e))
            block.gpsimd(lambda e: run_engine("pool", e))
            block.sync(lambda e: run_engine("sp", e))


class Builder:
    def __init__(self, stage=99, dbg=False):
        self.stage = stage
        self.dbg = dbg
        self.nc = bass.Bass("TRN2", target_bir_lowering=False)
        self.T = Tracker()
        self.T.new_chans("w", 6)
        self.T.new_chans("x", 4)
        self.T.new_chans("st", 4)
        self.T.new_chans("misc", 4)
        self.T.new_chans("ind", 8)
        self.pb_i = 0
        self.bregs = {}

    def mm(self, out, lhsT, rhs, start=True, stop=True):
        self.T.add("pe", lambda e: e.matmul(out, lhsT, rhs, start=start, stop=stop), [lhsT, rhs], [out])

    def tr(self, out, in_, ident):
        self.T.add("pe", lambda e: e.transpose(out, in_, ident), [in_, ident], [out])

    def act(self, out, in_, func, bias=None, scale=None, accum_out=None):
        ins = [in_]
        kw = {}
        if bias is not None:
            kw["bias"] = bias
            if not isinstance(bias, float):
                ins.append(bias)
        if scale is not None:
            kw["scale"] = scale
            if not isinstance(scale, float):
                ins.append(scale)
        outs = [out]
        if accum_out is not None:
            kw["accum_out"] = accum_out
            outs.append(accum_out)
        self.T.add("act", lambda e: e.activation(out, in_, func, **kw), ins, outs)

    def ts(self, out, in0, s1, s2, op0, op1=None, eng="dve"):
        ins = [in0] + [s for s in (s1, s2) if s is not None and not isinstance(s, float)]
        if op1 is None:
            self.T.add(eng, lambda e: e.tensor_scalar(out, in0, s1, None, op0), ins, [out])
        else:
            self.T.add(eng, lambda e: e.tensor_scalar(out, in0, s1, s2, op0, op1), ins, [out])

    def tt(self, out, in0, in1, op, eng="dve"):
        self.T.add(eng, lambda e: e.tensor_tensor(out, in0, in1, op), [in0, in1], [out])

    def stt(self, out, in0, scalar, in1, op0, op1, eng="dve"):
        ins = [in0, in1] + ([] if isinstance(scalar, float) else [scalar])
        self.T.add(eng, lambda e: e.scalar_tensor_tensor(out, in0, scalar, in1, op0, op1), ins, [out])

    def recip(self, out, in_):
        self.T.add("dve", lambda e: e.reciprocal(out, in_), [in_], [out])

    def cp(self, out, in_, eng="dve"):
        self.T.add(eng, lambda e: e.tensor_copy(out, in_), [in_], [out])

    def memset(self, out, v, eng="dve"):
        self.T.add(eng, lambda e: e.memset(out, v), [], [out])

    def dma(self, out, in_, eng="sp", group="misc", track_in=True, track_out=True, extra=()):
        return self.T.add(eng, lambda e: e.dma_start(out=out, in_=in_), [in_] if track_in else [],
                          [out] if track_out else [], chan_group=group, extra=extra)

    def _breg(self, e, bound):
        if bound not in self.bregs:
            self.bregs[bound] = e.to_reg(bound)
        return self.bregs[bound]

    def scatter(self, dram, idx_ap, src, bound, extra=()):
        return self.T.add("pool", lambda e: e.indirect_dma_start(
            out=dram, out_offset=bass.IndirectOffsetOnAxis(ap=idx_ap, axis=0), in_=src, in_offset=None,
            bounds_check=self._breg(e, bound), oob_is_err=False), [src, idx_ap], [], chan_group="ind", extra=extra)

    def gather(self, dst, dram, idx_ap, bound, extra=()):
        return self.T.add("pool", lambda e: e.indirect_dma_start(
            out=dst, out_offset=None, in_=dram, in_offset=bass.IndirectOffsetOnAxis(ap=idx_ap, axis=0),
            bounds_check=self._breg(e, bound), oob_is_err=False), [idx_ap], [dst], chan_group="ind", extra=extra)

    def bank(self):
        b = self.banks[self.pb_i % 8]
        self.pb_i += 1
        return b

    def abytes(self, off, nbytes, dt, shape=None):
        assert off % 4 == 0 and nbytes % 4 == 0 and off + nbytes <= self.arena_bytes, (off, nbytes)
        v = self.arena[:, off // 4:(off + nbytes) // 4]
        if dt == BF16:
            v = v.bitcast(BF16)
        if dt == I32:
            v = v.bitcast(I32)
        if shape is not None and len(shape) == 2:
            v = v.rearrange("p (a b) -> p a b", b=shape[1])
        elif shape is not None and len(shape) == 3:
            v = v.rearrange("p (a b c) -> p a b c", b=shape[1], c=shape[2])
        return v


def build(stage=99, dbg=False, sub=99):
    B = Builder(stage, dbg)
    stage_sub = sub
    nc = B.nc
    T = B.T

    def din(name, shape, dt=F32):
        return nc.dram_tensor(name, list(shape), dt, kind="ExternalInput").ap()

    xp = din("xp", [512, D]); xs = din("xs", [1280, D])
    ck = din("ck", [512, 512]); cv = din("cv", [512, 512])
    cond = din("cond", [32, 128])
    w_ada = din("w_ada", [D, 6 * D]); b_ada = din("b_ada", [96, 128])
    gmix = din("gmix", [16, 128]); gffn = din("gffn", [16, 128]); convw = din("convw", [48, 128])
    qg = din("qg", [1, 128]); kg = din("kg", [1, 128]); sink = din("sink", [1, 16])
    w_in = din("w_in", [D, IN_W]); w_co = din("w_co", [D, D]); w_ao = din("w_ao", [D, D]); w_o = din("w_o", [D, D])
    rw_d = din("rw", [D, NE]); rb_d = din("rb", [1, NE])
    weg = din("weg", [NE, D, DE]); weu = din("weu", [NE, D, DE]); wed = din("wed", [NE, DE, D])
    wsg = din("wsg", [D, DE]); wsu = din("wsu", [D, DE]); wsd = din("wsd", [DE, D])
    ident_d = din("ident", [128, 128]); prot_d = din("prot", [128, 128]); masks_d = din("masks", [4, 128, 128])
    cos_d = din("cosT", [128, 1280]); sin_d = din("sinT", [128, 1280]); flags_d = din("flags", [128, 2])
    ustr_d = din("ustr", [128, 128]); cnt0_d = din("cnt0", [128, 64]); lim_d = din("lim", [128, 64])
    XS = nc.dram_tensor("XS", [NSL, D], BF16, kind="Internal").ap()
    YS = nc.dram_tensor("YS", [NSL + 128, D], BF16, kind="Internal").ap()

    def dout(name, shape):
        return nc.dram_tensor(name, list(shape), F32, kind="ExternalOutput").ap()

    yp = dout("yp", [512, D]); ys = dout("ys", [1024, D]); nk = dout("nk", [512, 512]); nv = dout("nv", [512, 512])
    x1s = nc.dram_tensor("x1s", [1536, D], F32, kind="Internal").ap()
    if dbg:
        dbg_o = dout("dbg", [128, 4096])

    import contextlib
    with contextlib.ExitStack() as st:
        ARENA_F32 = 52000
        B.arena_bytes = ARENA_F32 * 4
        B.arena = st.enter_context(nc.sbuf_tensor("ar", [128, ARENA_F32], F32))
        B.banks = [st.enter_context(nc.psum_tensor("pb%d" % i, [128, 512], F32)) for i in range(8)]
        B.banks = [b[:, :] for b in B.banks]

        cur = [0]

        def alloc(nbytes, dt, shape=None):
            nbytes = (nbytes + 3) // 4 * 4
            v = B.abytes(cur[0], nbytes, dt, shape)
            cur[0] += nbytes
            return v

        ident = alloc(128 * 4, F32)
        ones_bf = alloc(128 * 2, BF16)
        prot_bf = alloc(128 * 2, BF16)
        masks_bf = alloc(4 * 128 * 2, BF16, (4, 128))
        featT = alloc(112 * 4, F32)
        baT = alloc(96 * 4, F32)
        scond = alloc(32 * 2, BF16)
        modsT = alloc(96 * 2 * 4, F32, (96, 2))
        A1 = alloc(32 * 4, F32, (16, 2)); A2 = alloc(32 * 4, F32, (16, 2))
        gqs = alloc(4, F32); gks = alloc(4, F32)
        esink = alloc(16 * 4, F32)
        kg_bc = alloc(128 * 4, F32)
        rbias = alloc(64 * 4, F32)
        flags = alloc(2 * 4, F32)
        rw = alloc(16 * 64 * 4, F32, (16, 64))
        gt_bc = alloc(D * 2, BF16)
        small = alloc(64 * 4, F32)
        ustr_bf = alloc(128 * 2, BF16)
        cntb = alloc(64 * 4, F32)
        limc = alloc(64 * 4, F32)
        rows_i = alloc(12 * 8 * 4, I32, (12, 8))
        w_all = alloc(12 * 8 * 4, F32, (12, 8))
        A2bc = alloc(D * 2, BF16)
        B2bc = alloc(D * 2, BF16)
        alloc_eps = alloc(4, F32)
        const_end = cur[0]

        NSLOT = 5
        wslots = [alloc(16 * 256 * 2, BF16) for _ in range(NSLOT)]
        slot_i = [0]
        phase_base = cur[0]

        def wload_cols(src, c0, ncols=256, rows=D):
            s = wslots[slot_i[0] % NSLOT]
            slot_i[0] += 1
            kc = rows // 128
            v = s[:, 0:kc * ncols].rearrange("p (k c) -> p k c", c=ncols)
            B.dma(v, src[:, c0:c0 + ncols].rearrange("(k p) c -> p k c", p=128), eng="pool", group="w", track_in=False)
            return v

        B.dma(ident, ident_d, group="misc", track_in=False)
        B.memset(ones_bf, 1.0)
        B.dma(prot_bf, prot_d, eng="pool", group="misc", track_in=False)
        B.dma(masks_bf, masks_d.rearrange("m p q -> p m q"), eng="pool", group="misc", track_in=False)
        B.dma(flags, flags_d, group="misc", track_in=False)
        B.dma(ustr_bf, ustr_d, eng="pool", group="misc", track_in=False)
        B.dma(cntb, cnt0_d, group="misc", track_in=False)
        B.dma(limc, lim_d, group="misc", track_in=False)
        B.dma(rw, rw_d.rearrange("(k p) e -> p k e", p=128), group="misc", track_in=False)
        B.dma(rbias, rb_d.partition_broadcast(128), group="misc", track_in=False)
        B.dma(kg_bc, kg.partition_broadcast(128), group="misc", track_in=False)
        B.dma(esink, sink.partition_broadcast(128), group="misc", track_in=False)
        B.act(esink, esink, AF.Exp)
        B.dma(gqs, qg.rearrange("o p -> p o"), group="misc", track_in=False)
        B.dma(gks, kg.rearrange("o p -> p o"), group="misc", track_in=False)
        B.ts(gqs, gqs, float(128.0 ** -0.5), None, ALU.mult)
        eps_ap = alloc_eps
        B.memset(eps_ap, EPS)

        cur[0] = phase_base
        stackA = alloc(128 * 4, F32)
        stackB = alloc(128 * 4, F32)
        B.dma(stackA[0:32, :], cond, group="misc", track_in=False)
        B.dma(stackA[32:48, :], gmix, group="misc", track_in=False)
        B.dma(stackA[48:64, :], gffn, group="misc", track_in=False)
        B.dma(stackA[64:112, :], convw, group="misc", track_in=False)
        B.dma(stackB[0:96, :], b_ada, group="misc", track_in=False)
        pb = B.bank()
        B.tr(pb[:, 0:112], stackA[0:112, :], ident[0:112, 0:112])
        B.cp(featT, pb[:, 0:112])
        pb = B.bank()
        B.tr(pb[:, 0:96], stackB[0:96, :], ident[0:96, 0:96])
        B.cp(baT, pb[:, 0:96])
        B.act(scond, featT[:, 0:32], AF.Silu)
        pm = B.bank()
        for blk in range(48):
            wv = wload_cols(w_ada, blk * 256)
            for fc in range(2):
                c = blk * 2 + fc
                for k in range(16):
                    B.mm(pm[:, c * 2:c * 2 + 2], wv[:, k, fc * 128:(fc + 1) * 128],
                         scond.rearrange("p (r k) -> p k r", k=16)[:, k, :], start=(k == 0), stop=(k == 15))
        B.tt(modsT, pm[:, 0:192].rearrange("p (c r) -> p c r", r=2),
             baT.unsqueeze(2).to_broadcast([128, 96, 2]), ALU.add)
        B.ts(A1, modsT[:, 16:32, :], 1.0, None, ALU.add)
        B.tt(A1, A1, featT[:, 32:48].unsqueeze(2).to_broadcast([128, 16, 2]), ALU.mult)
        B.ts(A2, modsT[:, 64:80, :], 1.0, None, ALU.add)
        B.tt(A2, A2, featT[:, 48:64].unsqueeze(2).to_broadcast([128, 16, 2]), ALU.mult)
        B1 = modsT[:, 0:16, :]
        B2 = modsT[:, 48:64, :]
        convT = featT[:, 64:112]
        halo_flags = flags[:, 0:2]

        def make_bc(dst, colfn):
            gtmp = alloc_tmp_f32
            for k in range(16):
                B.cp(gtmp, colfn(k).to_broadcast([128, 128]))
                if k % 4 == 0:
                    pbk = B.bank()
                B.mm(pbk[:, (k % 4) * 128:(k % 4 + 1) * 128], gtmp, ident)
                if k % 4 == 3:
                    B.act(dst[:, (k - 3) * 128:(k + 1) * 128], pbk, AF.Copy)

        def make_gate_bc(mod_idx, r):
            make_bc(gt_bc, lambda k: modsT[:, mod_idx * 16 + k, r:r + 1])

        cur[0] = phase_base
        alloc_tmp_f32 = alloc(128 * 4, F32)
        xt = [alloc(D * 4, F32) for _ in range(2)]
        xn = alloc(D * 4, F32)
        junk = alloc(D * 2, BF16)
        hT_off = cur[0]
        hT = alloc(16 * 512 * 2, BF16, (16, 512))
        Zoff = cur[0]
        zT = alloc(16 * 512 * 2, BF16, (16, 512))
        hTh = B.abytes(Zoff, 16 * 256 * 2, BF16, (16, 256))
        attnT = zT
        qT = alloc(16 * 512 * 2, BF16, (16, 512))
        acc_off = cur[0]
        kT = alloc(4 * 768 * 2, BF16, (4, 768))
        vbf = alloc(6 * 512 * 2, BF16, (6, 512))
        t1 = alloc(16 * 512 * 2, BF16, (16, 512))
        pT = alloc(7 * 512 * 2, BF16, (7, 512))
        ckT = alloc(4 * 512 * 2, BF16, (4, 512))
        cvb = alloc(4 * 512 * 2, BF16, (4, 512))
        hTe = alloc(16 * 2 * 2, BF16, (16, 2))
        aT_off = cur[0]
        cosb = alloc(768 * 4, F32); sinb = alloc(768 * 4, F32)
        wk1 = alloc(520 * 4, F32); wk2 = alloc(520 * 4, F32); wk3 = alloc(520 * 4, F32)
        sgb = alloc(512 * 2, BF16)
        qn = alloc(512 * 2, BF16)
        sqb = alloc(512 * 2, BF16)
        rst = alloc(512 * 4, F32)
        cks = xt[0].rearrange("p (b c) -> p b c", c=512)
        mphase_end = cur[0]
        x1 = B.abytes(phase_base + 128 * 4 + 2 * D * 4 + D * 4 + D * 2, 4 * D * 4, F32, (4, D))

        def norm_tile(src_rows, Aap, Bap, r, dst, dst_cols, xslot, also_f32=None):
            if xslot is None:
                x_ = src_rows
            else:
                x_ = xt[xslot]
                B.dma(x_, src_rows, eng="sp", group="x", track_in=True)
            ssq = small[:, 0:1]
            rstd = small[:, 1:2]
            B.act(xn, x_, AF.Square)
            B.T.add("dve", (lambda e, o_=ssq, i_=xn: e.tensor_reduce(o_, i_, AX.X, ALU.add)), [xn], [ssq])
            B.act(rstd, ssq, AF.Sqrt, bias=eps_ap, scale=1.0 / D)
            B.recip(rstd, rstd)
            B.act(xn, x_, AF.Copy, scale=rstd)
            for kb in range(4):
                pbk = B.bank()
                for j in range(4):
                    k = kb * 4 + j
                    B.tr(pbk[:, j * 128:(j + 1) * 128], xn[:, k * 128:(k + 1) * 128], ident)
                pv = pbk.rearrange("p (j t) -> p j t", t=128)
                a_b = Aap[:, kb * 4:kb * 4 + 4, r:r + 1].to_broadcast([128, 4, 128])
                b_b = Bap[:, kb * 4:kb * 4 + 4, r:r + 1].to_broadcast([128, 4, 128])
                if also_f32 is not None:
                    f = also_f32[:, kb * 4:kb * 4 + 4, :]
                    B.tt(f, pv, a_b, ALU.mult)
                    B.tt(f, f, b_b, ALU.add)
                    B.cp(dst[:, kb * 4:kb * 4 + 4, dst_cols], f)
                else:
                    tmpv = wk1[:, 0:512].rearrange("p (j t) -> p j t", t=128)
                    B.tt(tmpv, pv, a_b, ALU.mult)
                    B.tt(dst[:, kb * 4:kb * 4 + 4, dst_cols], tmpv, b_b, ALU.add)

        def yrows(G, i):
            if G["kind"] == "p":
                return yp[i * 128:(i + 1) * 128, :]
            o = G["base"]
            return ys[o + i * 128: o + (i + 1) * 128, :]

        zsrc = B.abytes(hT_off, 8 * D * 2, BF16, (8, D))
        B.memset(zsrc, 0.0, eng="pool")
        zero_ops = []
        for zi in range(NSL // 1024):
            zero_ops.append(B.dma(XS[zi * 1024:(zi + 1) * 1024, :].rearrange("(p a) d -> p a d", a=8), zsrc,
                                  eng="sp", group="st", track_out=False))
        zero_ops.append(B.dma(YS[NSL:NSL + 128, :], zsrc[:, 0, :], eng="sp", group="st", track_out=False))
        scat_ops = []

        groups = [
            dict(kind="p", r=0, rows=[xp[i * 128:(i + 1) * 128, :] for i in range(4)], x1row=0),
            dict(kind="s", r=1, base=0, x1row=512),
            dict(kind="s", r=1, base=512, x1row=1024),
        ]
        cache_ready = [False]

        for gi, G in enumerate(groups):
            if gi >= stage:
                break
            r = G["r"]
            samp = G["kind"] == "s"
            if samp:
                base = G["base"]
                main_rows = [xs[base + 128 + i * 128: base + 256 + i * 128, :] for i in range(4)]
                halo_rows = [xs[base: base + 128, :], xs[base + 640: base + 768, :]]
            else:
                main_rows = G["rows"]
                halo_rows = []
            make_gate_bc(2, r)
            if samp and not cache_ready[0]:
                cache_ready[0] = True
                B.dma(cks, ck.rearrange("(b p) c -> p b c", p=128), group="misc", track_in=False)
                B.dma(cvb, cv.rearrange("(b p) c -> p b c", p=128), eng="pool", group="misc", track_in=False)
                for b in range(4):
                    pbk = B.bank()
                    for kvh in range(4):
                        B.tr(pbk[:, kvh * 128:(kvh + 1) * 128], cks[:, b, kvh * 128:(kvh + 1) * 128], ident)
                    B.cp(ckT[:, :, b * 128:(b + 1) * 128], pbk.rearrange("p (h t) -> p h t", t=128))
            if samp:
                B.dma(cosb, cos_d[:, base:base + 768], group="misc", track_in=False)
                B.dma(sinb, sin_d[:, base:base + 768], group="misc", track_in=False)
            xs_i = 0
            for h, rows in enumerate(halo_rows):
                norm_tile(rows, A1, B1, r, hTh, slice(h * 128, (h + 1) * 128), xs_i % 2)
                xs_i += 1
            for i, rows in enumerate(main_rows):
                norm_tile(rows, A1, B1, r, hT, slice(i * 128, (i + 1) * 128), xs_i % 2)
                xs_i += 1
            if samp:
                B.cp(hTe[:, :, 0:1], hTh[:, :, 127:128])
                B.cp(hTe[:, :, 1:2], hTh[:, :, 128:129])

            koff = 128 if samp else 0

            def qk_head(ps, dst, gain, cos_cols, ncols):
                B.act(sqb[:, 0:ncols], ps, AF.Square)
                p2 = B.bank()
                B.mm(p2[:, 0:ncols], ones_bf, sqb[:, 0:ncols])
                B.act(rst[:, 0:ncols], p2[:, 0:ncols], AF.Sqrt, bias=eps_ap, scale=1.0 / 128)
                B.recip(rst[:, 0:ncols], rst[:, 0:ncols])
                if cos_cols is None:
                    B.stt(dst, ps, gain, rst[:, 0:ncols], ALU.mult, ALU.mult)
                else:
                    B.stt(qn[:, 0:ncols], ps, gain, rst[:, 0:ncols], ALU.mult, ALU.mult)
                    p3 = B.bank()
                    B.mm(p3[:, 0:ncols], prot_bf, qn[:, 0:ncols])
                    B.tt(wk2[:, 0:ncols], qn[:, 0:ncols], cosb[:, cos_cols], ALU.mult)
                    B.tt(wk3[:, 0:ncols], p3[:, 0:ncols], sinb[:, cos_cols], ALU.mult)
                    B.tt(dst, wk2[:, 0:ncols], wk3[:, 0:ncols], ALU.add)

            for half in range(2):
                wv = wload_cols(w_in, OFF_K + half * 256)
                for fc in range(2):
                    kvh = half * 2 + fc
                    ps = B.bank()
                    for k in range(16):
                        B.mm(ps, wv[:, k, fc * 128:(fc + 1) * 128], hT[:, k, :], start=(k == 0), stop=(k == 15))
                    qk_head(ps, kT[:, kvh, koff:koff + 512], gks, slice(128, 640) if samp else None, 512)
                    if samp:
                        ps = B.bank()
                        for k in range(16):
                            B.mm(ps[:, 0:256], wv[:, k, fc * 128:(fc + 1) * 128], hTh[:, k, :], start=(k == 0), stop=(k == 15))
                        qk_head(ps[:, 0:128], kT[:, kvh, 0:128], gks, slice(0, 128), 128)
                        qk_head(ps[:, 128:256], kT[:, kvh, 640:768], gks, slice(640, 768), 128)
                if not samp:
                    for i in range(4):
                        ps = B.bank()
                        for k in range(16):
                            B.mm(ps[:, 0:256], hT[:, k, i * 128:(i + 1) * 128], wv[:, k, :], start=(k == 0), stop=(k == 15))
                        for fc in range(2):
                            pss = ps[:, fc * 128:(fc + 1) * 128]
                            ssq = small[:, 2 + fc:3 + fc]
                            B.act(wk3[:, 0:128], pss, AF.Square)
                            B.T.add("dve", (lambda e, o_=ssq, i_=wk3[:, 0:128]: e.tensor_reduce(o_, i_, AX.X, ALU.add)), [wk3[:, 0:128]], [ssq])
                            B.act(ssq, ssq, AF.Sqrt, bias=eps_ap, scale=1.0 / 128)
                            B.recip(ssq, ssq)
                            B.stt(wk1[:, fc * 128:(fc + 1) * 128], pss, ssq, kg_bc, ALU.mult, ALU.mult)
                        B.dma(nk[i * 128:(i + 1) * 128, half * 256:(half + 1) * 256], wk1[:, 0:256], eng="sp", group="st")
            for half in range(2):
                wv = wload_cols(w_in, OFF_V + half * 256)
                vt = ([(hTh[:, :, 0:128], 0)] if samp else []) + [(hT[:, :, i * 128:(i + 1) * 128], i + (1 if samp else 0)) for i in range(4)] \
                    + ([(hTh[:, :, 128:256], 5)] if samp else [])
                for src_h, vi in vt:
                    ps = B.bank()
                    for k in range(16):
                        B.mm(ps[:, 0:256], src_h[:, k, :], wv[:, k, :], start=(k == 0), stop=(k == 15))
                    B.act(vbf[:, vi, half * 256:(half + 1) * 256], ps[:, 0:256], AF.Copy)
                    if not samp:
                        B.cp(wk2[:, 0:256], ps[:, 0:256])
                        B.dma(nv[vi * 128:(vi + 1) * 128, half * 256:(half + 1) * 256], wk2[:, 0:256], eng="sp", group="st")
            if stage_sub < 2:
                continue
            nseq, L = (1, 512) if samp else (2, 256)
            for half in range(8):
                wcc = wload_cols(w_in, OFF_CC + half * 256)
                wcx = wload_cols(w_in, OFF_CX + half * 256)
                wcb = wload_cols(w_in, OFF_CB + half * 256)
                for fc in range(2):
                    j = half * 2 + fc
                    pcc = B.bank(); pcx = B.bank(); pcb = B.bank()
                    for k in range(16):
                        B.mm(pcc, wcc[:, k, fc * 128:(fc + 1) * 128], hT[:, k, :], start=(k == 0), stop=(k == 15))
                    for k in range(16):
                        B.mm(pcx, wcx[:, k, fc * 128:(fc + 1) * 128], hT[:, k, :], start=(k == 0), stop=(k == 15))
                    for k in range(16):
                        B.mm(pcb, wcb[:, k, fc * 128:(fc + 1) * 128], hT[:, k, :], start=(k == 0), stop=(k == 15))
                    uext = wk1[:, 0:nseq * (L + 2)].rearrange("p (s l) -> p s l", l=L + 2)
                    B.act(wk2[:, 0:512], pcc, AF.Copy)
                    B.tt(uext[:, :, 1:L + 1], wk2[:, 0:512].rearrange("p (s l) -> p s l", l=L),
                         pcx.rearrange("p (s l) -> p s l", l=L), ALU.mult)
                    if samp:
                        ph = B.bank()
                        for k in range(16):
                            B.mm(ph[:, 0:2], wcc[:, k, fc * 128:(fc + 1) * 128], hTe[:, k, :], start=(k == 0), stop=(k == 15))
                        for k in range(16):
                            B.mm(ph[:, 2:4], wcx[:, k, fc * 128:(fc + 1) * 128], hTe[:, k, :], start=(k == 0), stop=(k == 15))
                        B.cp(wk3[:, 0:4], ph[:, 0:4])
                        B.tt(wk3[:, 4:6], wk3[:, 0:2], wk3[:, 2:4], ALU.mult)
                        B.tt(wk3[:, 4:6], wk3[:, 4:6], halo_flags, ALU.mult)
                        B.cp(uext[:, 0, 0:1], wk3[:, 4:5])
                        B.cp(uext[:, 0, L + 1:L + 2], wk3[:, 5:6])
                    else:
                        B.memset(uext[:, :, 0:1], 0.0)
                        B.memset(uext[:, :, L + 1:L + 2], 0.0)
                    acc = wk2[:, 0:512].rearrange("p (s l) -> p s l", l=L)
                    B.ts(acc, uext[:, :, 1:L + 1], convT[:, 16 + j:17 + j], None, ALU.mult)
                    B.stt(acc, uext[:, :, 0:L], convT[:, j:j + 1], acc, ALU.mult, ALU.add)
                    B.stt(acc, uext[:, :, 2:L + 2], convT[:, 32 + j:33 + j], acc, ALU.mult, ALU.add)
                    B.tt(zT[:, j, :], wk2[:, 0:512], pcb, ALU.mult)
            for half in range(8):
                wv = wload_cols(w_in, OFF_Q + half * 256)
                for fc in range(2):
                    hh = half * 2 + fc
                    ps = B.bank()
                    for k in range(16):
                        B.mm(ps, wv[:, k, fc * 128:(fc + 1) * 128], hT[:, k, :], start=(k == 0), stop=(k == 15))
                    qk_head(ps, qT[:, hh, :], gqs, slice(128, 640) if samp else None, 512)
            for half in range(8):
                wg = wload_cols(w_in, OFF_GC + half * 256)
                wc = wload_cols(w_co, half * 256)
                for fc in range(2):
                    j = half * 2 + fc
                    pg = B.bank(); py = B.bank()
                    for k in range(16):
                        B.mm(pg, wg[:, k, fc * 128:(fc + 1) * 128], hT[:, k, :], start=(k == 0), stop=(k == 15))
                    for k in range(16):
                        B.mm(py, wc[:, k, fc * 128:(fc + 1) * 128], zT[:, k, :], start=(k == 0), stop=(k == 15))
                    B.act(sgb, pg, AF.Sigmoid)
                    B.tt(t1[:, j, :], sgb, py, ALU.mult)
            if stage_sub < 3:
                continue
            for qi in range(4):
                for kvh in range(4):
                    blocks = []
                    if samp:
                        tile_g = (0 if G["base"] == 0 else 4) + qi
                        mP = 0 if tile_g == 0 else 1
                        mN = 3 if tile_g == 7 else 2
                        for o, m in ((0, mP), (1, None), (2, mN)):
                            c0 = qi * 128 + o * 128
                            blocks.append((kT[:, kvh, c0:c0 + 128], vbf[:, qi + o, kvh * 128:(kvh + 1) * 128], m))
                        for b in range(4):
                            blocks.append((ckT[:, kvh, b * 128:(b + 1) * 128], cvb[:, b, kvh * 128:(kvh + 1) * 128], None))
                    else:
                        s = qi // 2
                        for t in (2 * s, 2 * s + 1):
                            blocks.append((kT[:, kvh, t * 128:(t + 1) * 128], vbf[:, t, kvh * 128:(kvh + 1) * 128], None))
                    qv = qT[:, 4 * kvh:4 * kvh + 4, qi * 128:(qi + 1) * 128]
                    nb = len(blocks)
                    for bi, (kap, vap, m) in enumerate(blocks):
                        ps = B.bank()
                        B.mm(ps, kap, qv)
                        B.act(pT[:, bi, :], ps, AF.Exp)
                        if m is not None:
                            pv3 = pT[:, bi, :].rearrange("p (g t) -> p g t", t=128)
                            B.tt(pv3, pv3, masks_bf[:, m, :].unsqueeze(1).to_broadcast([128, 4, 128]), ALU.mult)
                    po = B.bank(); pd = B.bank()
                    for bi, (kap, vap, m) in enumerate(blocks):
                        B.mm(po, vap, pT[:, bi, :], start=(bi == 0), stop=(bi == nb - 1))
                    for bi in range(nb):
                        B.mm(pd, ones_bf, pT[:, bi, :], start=(bi == 0), stop=(bi == nb - 1))
                    for g in range(4):
                        hh = 4 * kvh + g
                        B.ts(rst[:, g * 128:(g + 1) * 128], pd[:, g * 128:(g + 1) * 128], esink[:, hh:hh + 1], None, ALU.add)
                    B.T.add("dve", (lambda e, o_=wk3[:, 0:512], i_=rst: e.reciprocal(o_, i_)), [rst], [wk3[:, 0:512]])
                    B.tt(attnT[:, 4 * kvh:4 * kvh + 4, qi * 128:(qi + 1) * 128], po.rearrange("p (g t) -> p g t", t=128),
                         wk3[:, 0:512].rearrange("p (g t) -> p g t", t=128), ALU.mult)
            for half in range(8):
                wg = wload_cols(w_in, OFF_GA + half * 256)
                wc = wload_cols(w_ao, half * 256)
                for fc in range(2):
                    j = half * 2 + fc
                    pg = B.bank(); py = B.bank()
                    for k in range(16):
                        B.mm(pg, wg[:, k, fc * 128:(fc + 1) * 128], hT[:, k, :], start=(k == 0), stop=(k == 15))
                    for k in range(16):
                        B.mm(py, wc[:, k, fc * 128:(fc + 1) * 128], attnT[:, k, :], start=(k == 0), stop=(k == 15))
                    B.act(sgb, pg, AF.Sigmoid)
                    B.tt(wk2[:, 0:512], sgb, py, ALU.mult)
                    B.tt(t1[:, j, :], wk2[:, 0:512], t1[:, j, :], ALU.add)
            if stage_sub < 4:
                continue
            for i, rows in enumerate(main_rows):
                B.dma(x1[:, i, :], rows, eng="sp", group="x", track_in=False)
            for nb_ in range(8):
                wv = wload_cols(w_o, nb_ * 256)
                for i in range(4):
                    ps = B.bank()
                    for k in range(16):
                        B.mm(ps[:, 0:256], t1[:, k, i * 128:(i + 1) * 128], wv[:, k, :], start=(k == 0), stop=(k == 15))
                    B.tt(wk2[:, 0:256], ps[:, 0:256], gt_bc[:, nb_ * 256:(nb_ + 1) * 256], ALU.mult)
                    B.tt(x1[:, i, nb_ * 256:(nb_ + 1) * 256], wk2[:, 0:256], x1[:, i, nb_ * 256:(nb_ + 1) * 256], ALU.add)
            if stage_sub < 5:
                for i in range(4):
                    B.dma(yrows(G, i), x1[:, i, :], eng="sp", group="st")
                continue
            make_gate_bc(5, r)
            make_bc(A2bc, lambda k: A2[:, k, r:r + 1])
            make_bc(B2bc, lambda k: B2[:, k, r:r + 1])
            h2T = qT
            h2Tf = xt[1].rearrange("p (k t) -> p k t", t=128)
            accb = B.abytes(acc_off, 4 * D * 4, F32, (4, D))
            aT = B.abytes(aT_off, 4 * 512 * 2, BF16, (4, 512))
            h2tm = [junk, xt[0][:, 0:D // 2].bitcast(BF16)]
            for i in range(4):
                gt_ = gi * 4 + i
                norm_tile(x1[:, i, :], A2, B2, r, h2T, slice(i * 128, (i + 1) * 128), None, also_f32=h2Tf)
                hm = h2tm[i % 2]
                B.tt(xn, xn, A2bc, ALU.mult)
                B.tt(hm, xn, B2bc, ALU.add)
                pl = B.bank()
                for k in range(16):
                    B.mm(pl[:, 0:64], h2Tf[:, k, :], rw[:, k, :], start=(k == 0), stop=(k == 15))
                sc = wk1[:, 0:64]; bia = wk1[:, 64:128]; m1 = wk1[:, 128:136]; eq = wk1[:, 136:200]
                msk = wk1[:, 200:264]; m2 = wk1[:, 264:272]; gs = wk1[:, 272:280]; mx = wk1[:, 280:288]
                gsel = wk1[:, 288:296]; em = wk1[:, 296:360]; mx2 = wk1[:, 360:368]; sel = wk1[:, 368:432]
                ssum = wk1[:, 432:433]; selm = wk1[:, 440:504]
                gwt = wk2[:, 0:64]; rt = wk2[:, 64:128]; ov = wk2[:, 128:192]; r8 = wk2[:, 192:200]
                selb = sgb[:, 0:64]
                OH = wk3[:, 0:512].rearrange("p (k e) -> p k e", e=64)
                B.act(sc, pl[:, 0:64], AF.Sigmoid)
                B.tt(bia, sc, rbias, ALU.add)
                b3 = bia.rearrange("p (g j) -> p g j", j=8)
                B.T.add("dve", (lambda e, o_=m1, i_=b3: e.tensor_reduce(o_, i_, AX.X, ALU.max)), [bia], [m1])
                B.tt(eq.rearrange("p (g j) -> p g j", j=8), b3, m1.unsqueeze(2).to_broadcast([128, 8, 8]), ALU.is_equal)
                B.stt(msk, eq, -BIG, bia, ALU.mult, ALU.add)
                B.T.add("dve", (lambda e, o_=m2, i_=msk.rearrange("p (g j) -> p g j", j=8): e.tensor_reduce(o_, i_, AX.X, ALU.max)), [msk], [m2])
                B.tt(gs, m1, m2, ALU.add)
                B.T.add("dve", (lambda e, o_=mx, i_=gs: e.max(o_, i_)), [gs], [mx])
                B.ts(gsel, gs, mx[:, 3:4], None, ALU.is_ge)
                B.stt(em.rearrange("p (g j) -> p g j", j=8), b3, 2.0, gsel.unsqueeze(2).to_broadcast([128, 8, 8]), ALU.add, ALU.mult)
                B.T.add("dve", (lambda e, o_=mx2, i_=em: e.max(o_, i_)), [em], [mx2])
                B.ts(selm, em, mx2[:, 7:8], None, ALU.is_ge)
                B.tt(sel, selm, sc, ALU.mult)
                B.T.add("dve", (lambda e, o_=ssum, i_=sel: e.tensor_reduce(o_, i_, AX.X, ALU.add)), [sel], [ssum])
                B.recip(ssum, ssum)
                B.ts(gwt, sel, ssum, 2.5, ALU.mult, ALU.mult)
                B.cp(selb, selm)
                pp = B.bank()
                B.mm(pp[:, 0:64], ustr_bf, selb)
                B.mm(pp[:, 64:128], ones_bf, selb)
                B.tt(rt, pp[:, 0:64], cntb, ALU.add)
                B.tt(cntb, pp[:, 64:128], cntb, ALU.add)
                B.tt(ov, rt, limc, ALU.is_ge)
                B.stt(rt, ov, 1.0e6, rt, ALU.mult, ALU.add)
                B.ts(rt, rt, float(NSL), None, ALU.min)
                B.tt(OH, em.unsqueeze(1).to_broadcast([128, 8, 64]), mx2.unsqueeze(2).to_broadcast([128, 8, 64]), ALU.is_equal)
                OH2 = wk1[:, 0:512].rearrange("p (k e) -> p k e", e=64)
                OH2 = rst[:, 0:512].rearrange("p (k e) -> p k e", e=64)
                B.tt(OH2, OH, rt.unsqueeze(1).to_broadcast([128, 8, 64]), ALU.mult)
                B.T.add("dve", (lambda e, o_=r8, i_=OH2: e.tensor_reduce(o_, i_, AX.X, ALU.add)), [rst], [r8])
                B.cp(rows_i[:, gt_, :], r8)
                B.tt(OH2, OH, gwt.unsqueeze(1).to_broadcast([128, 8, 64]), ALU.mult)
                B.T.add("dve", (lambda e, o_=w_all[:, gt_, :], i_=OH2: e.tensor_reduce(o_, i_, AX.X, ALU.add)), [rst], [w_all[:, gt_, :]])
                for k8 in range(8):
                    scat_ops.append(B.scatter(XS, rows_i[:, gt_, k8:k8 + 1], hm, NSL - 1, extra=zero_ops))
            for half in range(2):
                wg = wload_cols(wsg, half * 256)
                wu = wload_cols(wsu, half * 256)
                for fc in range(2):
                    f = half * 2 + fc
                    pg = B.bank(); pu = B.bank()
                    for k in range(16):
                        B.mm(pg, wg[:, k, fc * 128:(fc + 1) * 128], h2T[:, k, :], start=(k == 0), stop=(k == 15))
                    for k in range(16):
                        B.mm(pu, wu[:, k, fc * 128:(fc + 1) * 128], h2T[:, k, :], start=(k == 0), stop=(k == 15))
                    B.act(rst, pg, AF.Silu)
                    B.tt(aT[:, f, :], rst, pu, ALU.mult)
            for dh in range(2):
                wd = wload_cols(wsd, dh * 1024, ncols=1024, rows=512)
                for i in range(4):
                    for n in range(2):
                        ps = B.bank()
                        for f in range(4):
                            B.mm(ps, aT[:, f, i * 128:(i + 1) * 128], wd[:, f, n * 512:(n + 1) * 512], start=(f == 0), stop=(f == 3))
                        cs = slice(dh * 1024 + n * 512, dh * 1024 + (n + 1) * 512)
                        B.tt(wk2[:, 0:512], ps, gt_bc[:, cs], ALU.mult)
                        B.tt(x1[:, i, cs], wk2[:, 0:512], x1[:, i, cs], ALU.add)
            for i in range(4):
                B.dma(x1s[gi * 512 + i * 128: gi * 512 + (i + 1) * 128, :], x1[:, i, :], eng="sp", group="st")

        if stage >= 3 and stage_sub >= 5:
            cur[0] = phase_base
            NXS = 12
            xslots = list(wslots) + [alloc(16 * 256 * 2, BF16) for _ in range(NXS - NSLOT)]
            xs_i = [0]

            def xload_cols(src, c0, ncols=256, rows=D):
                s = xslots[xs_i[0] % NXS]
                xs_i[0] += 1
                kc = rows // 128
                v = s[:, 0:kc * ncols].rearrange("p (k c) -> p k c", c=ncols)
                B.dma(v, src[:, c0:c0 + ncols].rearrange("(k p) c -> p k c", p=128), eng="pool", group="w", track_in=False)
                return v

            NT = (CAP + 127) // 128
            xe_tm = [alloc(NT * D * 2, BF16, (NT, D)) for _ in range(2)]
            xeT = [alloc(16 * CAP * 2, BF16, (16, CAP)) for _ in range(2)]
            aTe = [alloc(4 * CAP * 2, BF16, (4, CAP)) for _ in range(2)]
            sge = alloc(CAP * 4, F32)
            yeb = [alloc(D * 2, BF16) for _ in range(3)]
            ident_bf = alloc(128 * 2, BF16)
            B.cp(ident_bf, ident)
            ye_i = 0
            ystore_ops = []
            def xe_load(e2):
                for s in range(NT):
                    n_s = min(128, CAP - s * 128)
                    B.dma(xe_tm[e2 % 2][0:n_s, s, :], XS[e2 * CAP + s * 128: e2 * CAP + s * 128 + n_s, :], eng="sp", group="x",
                          track_in=False, extra=scat_ops + zero_ops)

            xe_load(0)
            for e_ in range(NE):
                xt_ = xe_tm[e_ % 2]
                xT_ = xeT[e_ % 2]
                aT_ = aTe[e_ % 2]
                if e_ + 1 < NE:
                    xe_load(e_ + 1)
                for s in range(NT):
                    n_s = min(128, CAP - s * 128)
                    for kb in range(2):
                        pbk = B.bank().bitcast(BF16)
                        for j in range(8):
                            k = kb * 8 + j
                            B.tr(pbk[:, j * 128:j * 128 + n_s], xt_[0:n_s, s, k * 128:(k + 1) * 128], ident_bf[0:n_s, 0:n_s])
                        src_v = pbk.rearrange("p (j t) -> p j t", t=128)[:, :, 0:n_s]
                        dst_v = xT_[:, kb * 8:kb * 8 + 8, s * 128:s * 128 + n_s]
                        if kb == 0:
                            B.act(dst_v, src_v, AF.Copy)
                        else:
                            B.cp(dst_v, src_v)
                for half in range(2):
                    wg = xload_cols(weg[e_], half * 256)
                    wu = xload_cols(weu[e_], half * 256)
                    for fc in range(2):
                        f = half * 2 + fc
                        pg = B.bank(); pu = B.bank()
                        for k in range(16):
                            B.mm(pg[:, 0:CAP], wg[:, k, fc * 128:(fc + 1) * 128], xT_[:, k, :], start=(k == 0), stop=(k == 15))
                        for k in range(16):
                            B.mm(pu[:, 0:CAP], wu[:, k, fc * 128:(fc + 1) * 128], xT_[:, k, :], start=(k == 0), stop=(k == 15))
                        B.act(sge, pg[:, 0:CAP], AF.Silu)
                        B.tt(aT_[:, f, :], sge, pu[:, 0:CAP], ALU.mult)
                wds = [xload_cols(wed[e_], dh * 1024, ncols=1024, rows=512) for dh in range(2)]
                for s in range(NT):
                    n_s = min(128, CAP - s * 128)
                    yb = yeb[ye_i % 3]
                    ye_i += 1
                    for dh in range(2):
                        for n in range(2):
                            ps = B.bank()
                            for f in range(4):
                                B.mm(ps[0:n_s, :], aT_[:, f, s * 128:s * 128 + n_s], wds[dh][:, f, n * 512:(n + 1) * 512],
                                     start=(f == 0), stop=(f == 3))
                            cs = slice(dh * 1024 + n * 512, dh * 1024 + (n + 1) * 512)
                            if n == 0:
                                B.act(yb[0:n_s, cs], ps[0:n_s, :], AF.Copy)
                            else:
                                B.cp(yb[0:n_s, cs], ps[0:n_s, :])
                    ystore_ops.append(B.dma(YS[e_ * CAP + s * 128: e_ * CAP + s * 128 + n_s, :], yb[0:n_s, :], eng="sp", group="st",
                                            track_out=False))
            cur[0] = phase_base
            accc = [alloc(D * 4, F32) for _ in range(2)]
            x1c = [alloc(D * 4, F32) for _ in range(2)]
            ygb = [alloc(D * 2, BF16) for _ in range(4)]
            yg_i = 0
            cur_r = [None]
            for gt_ in range(12):
                gi = gt_ // 4
                r = groups[gi]["r"]
                if cur_r[0] != r:
                    make_gate_bc(5, r)
                    cur_r[0] = r
                ac = accc[gt_ % 2]
                xc = x1c[gt_ % 2]
                B.dma(xc, x1s[gt_ * 128:(gt_ + 1) * 128, :], eng="sp", group="x")
                for k8 in range(8):
                    yg = ygb[yg_i % 4]
                    yg_i += 1
                    B.gather(yg, YS, rows_i[:, gt_, k8:k8 + 1], NSL, extra=ystore_ops + zero_ops)
                    if k8 == 0:
                        B.ts(ac, yg, w_all[:, gt_, 0:1], None, ALU.mult)
                    else:
                        B.stt(ac, yg, w_all[:, gt_, k8:k8 + 1], ac, ALU.mult, ALU.add)
                for q4 in range(4):
                    cs = slice(q4 * 512, (q4 + 1) * 512)
                    B.tt(ac[:, cs], ac[:, cs], gt_bc[:, cs], ALU.mult)
                    B.tt(ac[:, cs], ac[:, cs], xc[:, cs], ALU.add)
                B.dma(yrows(groups[gi], gt_ % 4), ac, eng="sp", group="st")

        T.emit(nc)
    return nc


def _rope_tables(n_tokens=4096):
    GRID_W = 64
    pos = np.arange(n_tokens)
    row = (pos // GRID_W).astype(np.float32)
    col = (pos % GRID_W).astype(np.float32)
    n_freq = HD // 4
    inv = (10000.0 ** (-np.arange(n_freq, dtype=np.float32) / n_freq)).astype(np.float32)
    ang = np.concatenate([row[:, None] * inv, col[:, None] * inv], axis=-1)
    cos = np.cos(ang).astype(np.float32).T
    sin = np.sin(ang).astype(np.float32).T
    return np.concatenate([cos, cos], 0), np.concatenate([sin, sin], 0)


def make_in_maps(inp):
    f = lambda a: np.ascontiguousarray(np.asarray(a, dtype=np.float32))
    x_prompt = f(inp["x_prompt"]); x_sample = f(inp["x_sample"])
    cosF, sinF = _rope_tables(4096)
    ident = np.eye(128, dtype=np.float32)
    prot = np.zeros((128, 128), np.float32)
    for m in range(64):
        prot[m + 64, m] = -1.0
        prot[m, m + 64] = 1.0
    kk = np.arange(128)[:, None]; qq = np.arange(128)[None, :]
    mP = (kk >= qq).astype(np.float32); mN = (kk <= qq).astype(np.float32)
    shared = dict(
        w_ada=f(inp["w_ada"][0]), b_ada=f(inp["b_ada"][0]).reshape(96, 128),
        gmix=f(inp["norm_mix_g"][0]).reshape(16, 128), gffn=f(inp["norm_ffn_g"][0]).reshape(16, 128),
        convw=f(inp["conv_w"][0]).reshape(48, 128), qg=f(inp["q_norm_g"][0]).reshape(1, 128),
        kg=f(inp["k_norm_g"][0]).reshape(1, 128), sink=f(inp["attn_sink"][0]).reshape(1, 16),
        w_in=f(inp["w_in"][0]), w_co=f(inp["w_conv_out"][0]), w_ao=f(inp["w_attn_out"][0]), w_o=f(inp["w_out"][0]),
        rw=f(inp["router_w"][0]), rb=f(inp["router_bias"][0]).reshape(1, 64),
        weg=f(inp["w_exp_gate"][0]), weu=f(inp["w_exp_up"][0]), wed=f(inp["w_exp_down"][0]),
        wsg=f(inp["w_sh_gate"][0]), wsu=f(inp["w_sh_up"][0]), wsd=f(inp["w_sh_down"][0]),
        ident=ident, prot=prot,
        ustr=(np.arange(128)[:, None] < np.arange(128)[None, :]).astype(np.float32),
        cnt0=np.tile((np.arange(64) * CAP).astype(np.float32)[None, :], (128, 1)),
        lim=np.tile(((np.arange(64) + 1) * CAP).astype(np.float32)[None, :], (128, 1)),
    )
    maps = []
    for c in range(NCORES):
        b, j = c // 4, c % 4
        xs = np.zeros((1280, D), np.float32)
        lo, hi = j * 1024 - 128, j * 1024 + 1152
        slo, shi = max(lo, 0), min(hi, 4096)
        xs[slo - lo: shi - lo] = x_sample[b, slo:shi]
        cosT = np.zeros((128, 1280), np.float32); sinT = np.zeros((128, 1280), np.float32)
        cosT[:, slo - lo: shi - lo] = cosF[:, slo:shi]; sinT[:, slo - lo: shi - lo] = sinF[:, slo:shi]
        masks = np.stack([mP * (1.0 if j > 0 else 0.0), mP, mN, mN * (1.0 if j < 3 else 0.0)]).astype(np.float32)
        flags = np.zeros((128, 2), np.float32); flags[:, 0] = 1.0 if j > 0 else 0.0; flags[:, 1] = 1.0 if j < 3 else 0.0
        m = dict(shared)
        m.update(
            xp=np.ascontiguousarray(x_prompt[2 * c:2 * c + 2].reshape(512, D)), xs=xs,
            ck=f(inp["cache_k"][b, 0]).reshape(512, 512), cv=f(inp["cache_v"][b, 0]).reshape(512, 512),
            cond=np.stack([f(inp["c_ctx"]), f(inp["c"][b])]).reshape(32, 128),
            masks=masks, cosT=cosT, sinT=sinT, flags=flags,
        )
        maps.append(m)
    return maps


def assemble(results):
    y_prompt = np.zeros((16, 256, D), np.float32); y_sample = np.zeros((2, 4096, D), np.float32)
    new_k = np.zeros((16, 1, 256, NKV, HD), np.float32); new_v = np.zeros((16, 1, 256, NKV, HD), np.float32)
    for c, r in enumerate(results):
        b, j = c // 4, c % 4
        y_prompt[2 * c:2 * c + 2] = np.asarray(r["yp"]).reshape(2, 256, D)
        y_sample[b, j * 1024:(j + 1) * 1024] = np.asarray(r["ys"])
        new_k[2 * c:2 * c + 2, 0] = np.asarray(r["nk"]).reshape(2, 256, NKV, HD)
        new_v[2 * c:2 * c + 2, 0] = np.asarray(r["nv"]).reshape(2, 256, NKV, HD)
    return y_prompt, y_sample, new_k, new_v


def kernel(**inputs):
    nc = build()
    maps = make_in_maps(inputs)
    res = run_bass_kernel_spmd(nc, maps, core_ids=list(range(NCORES)))
    return assemble(res.results)
```
